# Optimizing a Trainium2 kernel written in Bass

```python
import math
import jax, jax.numpy as jnp
from jax import lax
import numpy as np

D_MODEL = 1024
BATCH = 4
SEQ = 8192
DEPTH = 4

GRID_W = 64
CTX_LEN = 256
HEAD_DIM = 64
ROPE_THETA = 10000.0
Q_BLOCK = 128
NORM_EPS = 1e-6
CONV_CH = D_MODEL // 2
CONV_WIDTH = 31
GQA_Q_HEADS = 8
GQA_KV_HEADS = 2
DIFF_HEADS = 4
DIFF_V_DIM = 2 * HEAD_DIM
N_BRANCH = 3
N_EXPERTS = 32
TOP_K = 4
EXPERT_FF = D_MODEL
SWIGLU_ALPHA = 1.702
SWIGLU_LIMIT = 7.0
EXPERT_BLOCK = 256

GQA_Q = GQA_Q_HEADS * HEAD_DIM
GQA_KV = GQA_KV_HEADS * HEAD_DIM
DIFF_QK = DIFF_HEADS * 2 * HEAD_DIM
DIFF_V = DIFF_HEADS * DIFF_V_DIM
KV_COLS = 2 * GQA_KV + DIFF_QK + DIFF_V
Q_COLS = GQA_Q + DIFF_QK
CONV_COLS = 2 * CONV_CH
GATE_COLS = N_BRANCH * D_MODEL
D_IN = KV_COLS + Q_COLS + CONV_COLS + GATE_COLS
KV_SPLITS = (GQA_KV, 2 * GQA_KV, 2 * GQA_KV + DIFF_QK)

kernel_name = "hybrid_conv_gqa_diffattn_moe_dit"


def _rmsnorm(x, g):
    xf = x.astype(jnp.float32)
    y = xf * lax.rsqrt(jnp.mean(xf * xf, axis=-1, keepdims=True) + NORM_EPS)
    return (y * g.astype(jnp.float32)).astype(x.dtype)


def _layernorm(x, g, b):
    xf = x.astype(jnp.float32)
    mu = jnp.mean(xf, axis=-1, keepdims=True)
    xc = xf - mu
    var = jnp.mean(xc * xc, axis=-1, keepdims=True)
    return (xc * lax.rsqrt(var + NORM_EPS) * g.astype(jnp.float32) + b.astype(jnp.float32)).astype(x.dtype)


def _modulated_norm(x, g, shift, scale):
    return _rmsnorm(x, g) * (1 + scale) + shift


def _axial_rope_tables(n_tok, dtype):
    rows = n_tok // GRID_W
    t_row = jnp.repeat(jnp.arange(rows, dtype=jnp.float32), GRID_W)
    t_col = jnp.tile(jnp.arange(GRID_W, dtype=jnp.float32), rows)
    n_freq = HEAD_DIM // 4
    inv_freq = ROPE_THETA ** (-jnp.arange(n_freq, dtype=jnp.float32) / n_freq)
    ang_r = t_row[:, None] * inv_freq[None, :]
    ang_c = t_col[:, None] * inv_freq[None, :]
    return tuple(a[:, None, :].astype(dtype) for a in (jnp.cos(ang_r), jnp.sin(ang_r), jnp.cos(ang_c), jnp.sin(ang_c)))


def _rot_half(x, cos, sin):
    x1, x2 = jnp.split(x, 2, axis=-1)
    return jnp.concatenate([x1 * cos - x2 * sin, x2 * cos + x1 * sin], axis=-1)


def _axial_rope(x, rope):
    cr, sr, cc, sc = rope
    xr, xc = jnp.split(x, 2, axis=-1)
    return jnp.concatenate([_rot_half(xr, cr, sr), _rot_half(xc, cc, sc)], axis=-1)


def _sweep_query_blocks(fn, q):
    b, n = q.shape[:2]
    nb = n // Q_BLOCK
    qb = jnp.moveaxis(q.reshape(b, nb, Q_BLOCK, *q.shape[2:]), 1, 0)
    out = lax.map(fn, qb)
    return jnp.moveaxis(out, 0, 1).reshape(b, n, *out.shape[3:])


def _gqa(q, k, v):
    b, nq = q.shape[:2]
    qg = q.reshape(b, nq, GQA_KV_HEADS, GQA_Q_HEADS // GQA_KV_HEADS, HEAD_DIM)
    s = jnp.einsum('bqhgd,bkhd->bhgqk', qg, k).astype(jnp.float32) * (HEAD_DIM ** -0.5)
    p = jax.nn.softmax(s, axis=-1).astype(v.dtype)
    o = jnp.einsum('bhgqk,bkhd->bqhgd', p, v)
    return o.reshape(b, nq, GQA_Q)


def _diff_attn(q, k, v, lam):
    s = jnp.einsum('bqhcd,bkhcd->bhcqk', q, k).astype(jnp.float32) * (HEAD_DIM ** -0.5)
    p = jax.nn.softmax(s, axis=-1)
    a = (p[:, :, 0] - lam * p[:, :, 1]).astype(v.dtype)
    return jnp.einsum('bhqk,bkhe->bqhe', a, v)


def _keys_values(z_kv, rope, gk_g, dk_g):
    b, n = z_kv.shape[:2]
    gk, gv, dk, dv = jnp.split(z_kv, KV_SPLITS, axis=-1)
    gk = _rmsnorm(gk.reshape(b, n, GQA_KV_HEADS, HEAD_DIM), gk_g)
    dk = _rmsnorm(dk.reshape(b, n, 2 * DIFF_HEADS, HEAD_DIM), dk_g)
    if rope is not None:
        gk = _axial_rope(gk, rope)
        dk = _axial_rope(dk, rope)
    return (gk, gv.reshape(b, n, GQA_KV_HEADS, HEAD_DIM),
            dk.reshape(b, n, DIFF_HEADS, 2, HEAD_DIM), dv.reshape(b, n, DIFF_HEADS, DIFF_V_DIM))


def _conformer_conv(a, dw_w, dw_b, ln_g, ln_b, pw_w):
    u = a[..., :CONV_CH] * jax.nn.sigmoid(a[..., CONV_CH:])
    u = lax.conv_general_dilated(u, dw_w[:, None, :], window_strides=(1,),
                                 padding=((CONV_WIDTH // 2, CONV_WIDTH // 2),),
                                 dimension_numbers=('NWC', 'WIO', 'NWC'),
                                 feature_group_count=CONV_CH) + dw_b
    u = jax.nn.silu(_layernorm(u, ln_g, ln_b))
    return u @ pw_w


def _mixer_output(z, kv, rope, lam, lam_init, gq_g, dq_g, subln_g, dw_w, dw_b, ln_g, ln_b, pw_w,
                  w_gqa_o, w_diff_o, b_gate, w_out):
    b, n = z.shape[:2]
    gk, gv, dk, dv = kv
    zq = z[..., KV_COLS:KV_COLS + Q_COLS]
    a = z[..., KV_COLS + Q_COLS:KV_COLS + Q_COLS + CONV_COLS]
    gate_pre = z[..., KV_COLS + Q_COLS + CONV_COLS:]
    gq = _rmsnorm(zq[..., :GQA_Q].reshape(b, n, GQA_Q_HEADS, HEAD_DIM), gq_g)
    dq = _rmsnorm(zq[..., GQA_Q:].reshape(b, n, 2 * DIFF_HEADS, HEAD_DIM), dq_g)
    if rope is not None:
        gq = _axial_rope(gq, rope)
        dq = _axial_rope(dq, rope)
    dq = dq.reshape(b, n, DIFF_HEADS, 2, HEAD_DIM)
    o_g = _sweep_query_blocks(lambda qb: _gqa(qb, gk, gv), gq)
    o_d = _sweep_query_blocks(lambda qb: _diff_attn(qb, dk, dv, lam), dq)
    o_d = (_rmsnorm(o_d, subln_g) * (1.0 - lam_init)).reshape(b, n, DIFF_V)
    br_a = _conformer_conv(a, dw_w, dw_b, ln_g, ln_b, pw_w)
    br_b = o_g @ w_gqa_o
    br_c = o_d @ w_diff_o
    g = jax.nn.sigmoid(gate_pre + b_gate).reshape(b, n, N_BRANCH, D_MODEL)
    merged = g[:, :, 0] * br_a + g[:, :, 1] * br_b + g[:, :, 2] * br_c
    return merged @ w_out


def _moe(h, w_r, b_r, w1, b1, w2, b2):
    t, d = h.shape
    logits = (h @ w_r).astype(jnp.float32) + b_r.astype(jnp.float32)
    top_val, top_idx = lax.top_k(logits, TOP_K)
    comb = jax.nn.softmax(top_val, axis=-1)
    flat_e = top_idx.reshape(-1)
    order = jnp.argsort(flat_e)
    e_sorted = flat_e[order]
    tok_sorted = order // TOP_K
    w_sorted = comb.reshape(-1)[order]
    counts = jnp.bincount(flat_e, length=N_EXPERTS)
    padded = (counts + EXPERT_BLOCK - 1) // EXPERT_BLOCK * EXPERT_BLOCK
    pad_end = jnp.cumsum(padded)
    pad_start = pad_end - padded
    start = jnp.cumsum(counts) - counts
    n_assign = t * TOP_K
    slot = pad_start[e_sorted] + jnp.arange(n_assign) - start[e_sorted]
    n_blocks = -(-n_assign // EXPERT_BLOCK) + N_EXPERTS
    n_slots = n_blocks * EXPERT_BLOCK
    slot_tok = jnp.full((n_slots,), t, dtype=tok_sorted.dtype).at[slot].set(tok_sorted)
    h_pad = jnp.concatenate([h, jnp.zeros((1, d), h.dtype)], axis=0)
    xb = h_pad[slot_tok].reshape(n_blocks, EXPERT_BLOCK, d)
    blk_e = jnp.minimum(jnp.searchsorted(pad_end, jnp.arange(n_blocks) * EXPERT_BLOCK, side='right'), N_EXPERTS - 1)

    def expert_block(args):
        xe, e = args
        gu = xe @ w1[e] + b1[e]
        gate = jnp.minimum(gu[:, :EXPERT_FF], SWIGLU_LIMIT)
        lin = jnp.clip(gu[:, EXPERT_FF:], -SWIGLU_LIMIT, SWIGLU_LIMIT)
        act = (lin + 1) * (gate * jax.nn.sigmoid(gate * SWIGLU_ALPHA))
        return act @ w2[e] + b2[e]

    yb = lax.map(expert_block, (xb, blk_e)).reshape(n_slots, d)
    y_assign = yb[slot] * w_sorted[:, None].astype(yb.dtype)
    return jax.ops.segment_sum(y_assign, tok_sorted, num_segments=t)


def setup_inputs(seed: int = 0) -> dict:
    key = jax.random.key(seed)
    ks = iter(jax.random.split(key, 40))
    L, D, E, F = DEPTH, D_MODEL, N_EXPERTS, EXPERT_FF

    def nrm(shape, scale):
        return jax.random.normal(next(ks), shape, jnp.float32) * scale

    def gain(shape):
        return 1.0 + nrm(shape, 0.02)

    return {
        "x": nrm((BATCH, SEQ, D), 1.0),
        "c": nrm((BATCH, D), 1.0),
        "ctx": nrm((BATCH, CTX_LEN, D), 1.0),
        "c_ctx": nrm((D,), 1.0),
        "w_mod": nrm((L, D, 6 * D), 0.5 * D ** -0.5),
        "b_mod": nrm((L, 6 * D), 0.02),
        "norm1_g": gain((L, D)),
        "norm2_g": gain((L, D)),
        "w_in": nrm((L, D, D_IN), D ** -0.5),
        "gqa_q_norm": gain((L, HEAD_DIM)),
        "gqa_k_norm": gain((L, HEAD_DIM)),
        "diff_q_norm": gain((L, HEAD_DIM)),
        "diff_k_norm": gain((L, HEAD_DIM)),
        "lam_q1": nrm((L, HEAD_DIM), 0.1),
        "lam_k1": nrm((L, HEAD_DIM), 0.1),
        "lam_q2": nrm((L, HEAD_DIM), 0.1),
        "lam_k2": nrm((L, HEAD_DIM), 0.1),
        "diff_subln_g": gain((L, DIFF_V_DIM)),
        "conv_dw_w": nrm((L, CONV_WIDTH, CONV_CH), CONV_WIDTH ** -0.5),
        "conv_dw_b": nrm((L, CONV_CH), 0.02),
        "conv_ln_g": gain((L, CONV_CH)),
        "conv_ln_b": nrm((L, CONV_CH), 0.02),
        "conv_pw_w": nrm((L, CONV_CH, D), CONV_CH ** -0.5),
        "w_gqa_o": nrm((L, GQA_Q, D), GQA_Q ** -0.5),
        "w_diff_o": nrm((L, DIFF_V, D), DIFF_V ** -0.5),
        "b_gate": nrm((L, GATE_COLS), 0.02),
        "w_out": nrm((L, D, D), D ** -0.5),
        "router_w": nrm((L, D, E), D ** -0.5),
        "router_b": nrm((L, E), 0.01),
        "exp_w1": nrm((L, E, D, 2 * F), D ** -0.5),
        "exp_b1": nrm((L, E, 2 * F), 0.02),
        "exp_w2": nrm((L, E, F, D), F ** -0.5),
        "exp_b2": nrm((L, E, D), 0.02),
    }


def reference(x, c, ctx, c_ctx, w_mod, b_mod, norm1_g, norm2_g, w_in, gqa_q_norm, gqa_k_norm,
              diff_q_norm, diff_k_norm, lam_q1, lam_k1, lam_q2, lam_k2, diff_subln_g, conv_dw_w, conv_dw_b,
              conv_ln_g, conv_ln_b, conv_pw_w, w_gqa_o, w_diff_o, b_gate, w_out, router_w, router_b,
              exp_w1, exp_b1, exp_w2, exp_b2):
    b, n_lat, d = x.shape
    n_ctx = ctx.shape[1]
    rope = _axial_rope_tables(n_lat, x.dtype)
    for l in range(DEPTH):
        last = l == DEPTH - 1
        lam_init = 0.8 - 0.6 * math.exp(-0.3 * l)
        lam = (jnp.exp(jnp.sum(lam_q1[l].astype(jnp.float32) * lam_k1[l].astype(jnp.float32)))
               - jnp.exp(jnp.sum(lam_q2[l].astype(jnp.float32) * lam_k2[l].astype(jnp.float32))) + lam_init)
        mod = jax.nn.silu(c) @ w_mod[l] + b_mod[l]
        mod_c = jax.nn.silu(c_ctx) @ w_mod[l] + b_mod[l]
        sh1, sc1, g1, sh2, sc2, g2 = jnp.split(mod[:, None, :], 6, axis=-1)
        csh1, csc1, cg1, csh2, csc2, cg2 = jnp.split(mod_c, 6, axis=-1)
        branch_params = (diff_subln_g[l], conv_dw_w[l], conv_dw_b[l], conv_ln_g[l], conv_ln_b[l], conv_pw_w[l],
                         w_gqa_o[l], w_diff_o[l], b_gate[l], w_out[l])

        h = _modulated_norm(x, norm1_g[l], sh1, sc1)
        hc = _modulated_norm(ctx, norm1_g[l], csh1, csc1)
        zc = hc @ (w_in[l][:, :KV_COLS] if last else w_in[l])
        kv_c = _keys_values(zc[..., :KV_COLS], None, gqa_k_norm[l], diff_k_norm[l])
        z = h @ w_in[l]
        kv_l = _keys_values(z[..., :KV_COLS], rope, gqa_k_norm[l], diff_k_norm[l])
        kv_all = tuple(jnp.concatenate([u, v], axis=1) for u, v in zip(kv_c, kv_l))
        y = _mixer_output(z, kv_all, rope, lam, lam_init, gqa_q_norm[l], diff_q_norm[l], *branch_params)
        if not last:
            yc = _mixer_output(zc, kv_c, None, lam, lam_init, gqa_q_norm[l], diff_q_norm[l], *branch_params)
            ctx = ctx + cg1 * yc
        x = x + g1 * y

        h2 = _modulated_norm(x, norm2_g[l], sh2, sc2).reshape(b * n_lat, d)
        moe_args = (router_w[l], router_b[l], exp_w1[l], exp_b1[l], exp_w2[l], exp_b2[l])
        if last:
            f = _moe(h2, *moe_args)
            x = x + g2 * f.reshape(b, n_lat, d)
        else:
            h2c = _modulated_norm(ctx, norm2_g[l], csh2, csc2).reshape(b * n_ctx, d)
            f = _moe(jnp.concatenate([h2, h2c], axis=0), *moe_args)
            x = x + g2 * f[:b * n_lat].reshape(b, n_lat, d)
            ctx = ctx + cg2 * f[b * n_lat:].reshape(b, n_ctx, d)
    return x
```

```python
import math
import numpy as np
import ml_dtypes
import concourse.bass as bass
import concourse.mybir as mybir
from concourse.bass_utils import run_bass_kernel_spmd

F32 = mybir.dt.float32
BF16 = mybir.dt.bfloat16
AF = mybir.ActivationFunctionType
ALU = mybir.AluOpType
AX = mybir.AxisListType

D = 1024
CTX = 256
NE = 32
D_IN = 6400
EPS = 1e-6
NPV = 48 * 2 + 8 + 8 + 24 + 4 + 4 + 4 + 124 + 4 + 1 + 4 + 512 + 512 + 2
O_BMOD = 0
O_N1 = 96
O_N2 = 104
O_BG = 112
O_DWB = 136
O_LNG = 140
O_LNB = 144
O_DW = 148
O_HG = 272
O_SUB = 276
O_LAM = 277
O_B1 = 281
O_B1P = 793
O_LI = 1305


class Sched:
    NDMA = 24

    def __init__(self, nc):
        self.nc = nc
        self.ops = []
        self.state = {}
        self.base = 0
        self.eng_ops = {e: [] for e in ("sp", "act", "pe", "dve", "pool")}

    def _conf(self, key):
        root = key[0]
        d = self.state.setdefault(root, {})
        out = []
        for k2 in d:
            m = min(len(k2), len(key))
            if k2[:m] == key[:m]:
                out.append(k2)
        return d, out

    def op(self, eng, fn, r=(), w=(), dma=False):
        idx = len(self.ops)
        deps = set()
        for key in r:
            d, ks = self._conf(key)
            for k2 in ks:
                lw = d[k2][0]
                if lw is not None:
                    deps.add(lw)
        for key in w:
            d, ks = self._conf(key)
            for k2 in ks:
                lw, rd = d[k2]
                if lw is not None:
                    deps.add(lw)
                deps.update(rd)
        for key in r:
            d = self.state[key[0]]
            d.setdefault(key, [None, []])[1].append(idx)
        for key in w:
            d, ks = self._conf(key)
            for k2 in ks:
                if k2 != key:
                    d[k2] = [idx, []]
            d[key] = [idx, []]
        deps.discard(idx)
        self.ops.append([eng, fn, deps, dma])
        self.eng_ops[eng].append(idx)
        return idx

    def barrier(self):
        n = len(self.ops)
        last = {}
        for e, l in self.eng_ops.items():
            last[e] = None
            for i in reversed(l):
                if self.ops[i][1] is not None:
                    last[e] = i
                    break
        dmas = [i for i in range(self.base, n) if self.ops[i][3]]
        for e in self.eng_ops:
            deps = set([i for i in dmas if self.ops[i][0] == "pool"][-self.NDMA:])
            deps.update([i for i in dmas if self.ops[i][0] != "pool"][-self.NDMA:])
            for e2, li in last.items():
                if li is not None and li >= self.base:
                    deps.add(li)
            self.ops.append([e, None, deps, False])
            self.eng_ops[e].append(len(self.ops) - 1)
        self.state = {}
        self.base = n

    def emit(self):
        nc = self.nc
        ops = self.ops
        n = len(ops)
        dma_slot = {}
        NP = 6
        for (sel_pool, base_, cnt_) in ((True, 0, NP), (False, NP, self.NDMA - NP)):
            ids = [i for i in range(n) if ops[i][3] and ((ops[i][0] == "pool") == sel_pool)]
            for k, i in enumerate(ids):
                dma_slot[i] = (base_ + k % cnt_, 16 * (k // cnt_ + 1))
                if k >= cnt_:
                    ops[i][2].add(ids[k - cnt_])
        sig = [False] * n
        for i in range(n):
            e = ops[i][0]
            for d in ops[i][2]:
                if not ops[d][3] and (ops[d][0] != e or e != "pe"):
                    sig[d] = True
        sigidx = [0] * n
        cnt = {e: 0 for e in self.eng_ops}
        for i in range(n):
            if sig[i]:
                cnt[ops[i][0]] += 1
                sigidx[i] = cnt[ops[i][0]]
        import contextlib
        with contextlib.ExitStack() as st:
            esem = {e: st.enter_context(nc.semaphore("s_" + e)) for e in self.eng_ops}
            dsem = [st.enter_context(nc.semaphore("d%d" % k)) for k in range(self.NDMA)]
            block = st.enter_context(nc.Block())

            def run(ename):
                def body(eng):
                    waited = {}
                    for i in self.eng_ops[ename]:
                        _, fn, deps, is_dma = ops[i]
                        need = {}
                        for d in deps:
                            if ops[d][3]:
                                k, v = dma_slot[d]
                                key = ("d", k)
                                sem = dsem[k]
                            elif ops[d][0] != ename or ename != "pe":
                                key = ("e", ops[d][0])
                                sem = esem[ops[d][0]]
                                v = sigidx[d]
                            else:
                                continue
                            if need.get(key, (None, 0))[1] < v:
                                need[key] = (sem, v)
                        for key, (sem, v) in need.items():
                            if waited.get(key, 0) < v:
                                eng.wait_ge(sem, v)
                                waited[key] = v
                        if fn is None:
                            continue
                        ins = fn(eng)
                        if is_dma:
                            ins.then_inc(dsem[dma_slot[i][0]], 16)
                        elif sig[i]:
                            ins.then_inc(esem[ename], 1)
                return body

            block.sync(run("sp"))
            block.scalar(run("act"))
            block.tensor(run("pe"))
            block.vector(run("dve"))
            block.gpsimd(run("pool"))


def build(S, DEPTH, LAYER_IDS=None):
    NT = S + CTX
    NG = S // 512
    groups = [(g * 512, 512, 0) for g in range(NG)] + [(S, CTX, 1)]
    NKT = NT // 128
    L = DEPTH
    nc = bass.Bass("TRN2", target_bir_lowering=False)

    def din(name, shape, dt=F32):
        return nc.dram_tensor(name, list(shape), dt, kind="ExternalInput").ap()

    xT_in = din("xT0", [D, NT])
    cT_in = din("cT", [128, 16])
    pv_in = din("pv", [L, 128, NPV])
    rb_in = din("rb", [L, 1, NE])
    b2_in = din("b2", [L, NE, D])
    w_mod = din("w_mod", [L, D, 6 * D])
    w_in = din("w_in", [L, D, D_IN])
    pw_w = din("conv_pw_w", [L, 512, D])
    wgo_w = din("w_gqa_o", [L, 512, D])
    wdo_w = din("w_diff_o", [L, 512, D])
    wout_w = din("w_out", [L, D, D])
    rw_w = din("router_w", [L, D, NE])
    w1_w = din("exp_w1", [L, NE, D, 2 * D])
    w2_w = din("exp_w2", [L, NE, D, D])
    cos_in = din("cosT", [128, S])
    sin_in = din("sinT", [128, S])
    cst_in = din("cst", [128, 4 * 128])
    sel_in = din("selc", [NE, NE * 128])
    out_d = nc.dram_tensor("outT", [D, NT], F32, kind="ExternalOutput").ap()

    xT = nc.dram_tensor("xT", [D, NT], F32).ap()
    kT = nc.dram_tensor("kT", [640, NT], BF16).ap()
    vD = nc.dram_tensor("vD", [NT, 768], BF16).ap()
    qT = nc.dram_tensor("qT", [D, NT], BF16).ap()
    uT = nc.dram_tensor("uT", [512, NT], BF16).ap()
    gT = nc.dram_tensor("gT", [3072, NT], F32).ap()
    ogT = nc.dram_tensor("ogT", [512, NT], BF16).ap()
    odT = nc.dram_tensor("odT", [512, NT], BF16).ap()

    import contextlib
    st = contextlib.ExitStack()
    ARENA = 48000
    arena = st.enter_context(nc.sbuf_tensor("arena", [128, ARENA], F32))
    arena_b = arena.bitcast(BF16)
    ps = [st.enter_context(nc.psum_tensor("ps%d" % i, [128, 512], F32)) for i in range(8)]
    ptr = [0]

    def alloc(shape, dt=F32):
        n = 1
        for s_ in shape[1:]:
            n *= s_
        words = n if dt == F32 else (n + 1) // 2
        words = (words + 15) // 16 * 16
        off = ptr[0]
        ptr[0] += words
        assert ptr[0] <= ARENA, ("SBUF arena overflow", ptr[0])
        if dt == F32:
            v = arena[:, off:off + n]
        else:
            v = arena_b[:, 2 * off:2 * off + n]
        if len(shape) == 3:
            v = v.rearrange("p (a b) -> p a b", a=shape[1])
        return v

    sc = Sched(nc)
    uid = [0]

    def U(prefix):
        uid[0] += 1
        return "%s%d" % (prefix, uid[0])

    def dma(q, out, in_, r, w):
        sc.op(q, lambda e: e.dma_start(out=out, in_=in_), r=r, w=w, dma=True)

    def mm(out, lhsT, rhs, start, stop, r, w):
        sc.op("pe", lambda e: e.matmul(out, lhsT, rhs, start=start, stop=stop), r=r, w=w)

    def act(out, in_, func, r, w, bias=None, scale=None):
        kw = {}
        if bias is not None:
            kw["bias"] = bias
        if scale is not None:
            kw["scale"] = scale
        sc.op("act", lambda e: e.activation(out, in_, func, **kw), r=r, w=w)

    def tt(eng, out, a, b, op, r, w):
        sc.op(eng, lambda e: e.tensor_tensor(out, a, b, op), r=r, w=w)

    def ts(eng, out, a, s1, s2, op0, op1, r, w):
        if s2 is None:
            sc.op(eng, lambda e: e.tensor_scalar(out, a, s1, None, op0), r=r, w=w)
        else:
            sc.op(eng, lambda e: e.tensor_scalar(out, a, s1, s2, op0, op1), r=r, w=w)

    def stt(out, a, s, b, op0, op1, r, w):
        sc.op("dve", lambda e: e.scalar_tensor_tensor(out, a, s, b, op0, op1), r=r, w=w)

    def recip(out, in_, r, w):
        sc.op("dve", lambda e: e.reciprocal(out, in_), r=r, w=w)

    cst = alloc([128, 512])
    dma("sp", cst, cst_in, r=[], w=[("cst",)])
    ident = cst[:, 0:128]
    blk64 = cst[:, 128:256]
    ones_f = cst[:, 256:384]
    Rt_f = cst[:, 384:512]
    cstb = alloc([128, 512], BF16)
    sc.op("dve", lambda e: e.tensor_copy(cstb, cst), r=[("cst",)], w=[("cstb",)])
    identb = cstb[:, 0:128]
    onesb = cstb[:, 256:384]
    Rtb = cstb[:, 384:512]
    selcb = alloc([128, NE * 128], BF16)
    selc3 = selcb.rearrange("p (e m) -> p e m", e=NE)
    dma("pool", selc3[0:NE, :, :], sel_in.rearrange("k (e m) -> k e m", e=NE), r=[], w=[("selc",)])
    epsT = alloc([128, 1])
    sc.op("dve", lambda e: e.memset(epsT, EPS), w=[("eps",)])
    cT = alloc([128, 16])
    dma("sp", cT, cT_in, r=[], w=[("cT",)])
    silc = alloc([128, 16])
    act(silc, cT, AF.Silu, r=[("cT",)], w=[("silc",)])
    pv = alloc([128, NPV])
    modT = alloc([128, 96])
    GS = alloc([128, 6 * 16])
    lamt = alloc([128, 4])
    rbb = alloc([128, NE], BF16)
    b2bb = alloc([128, D], BF16)
    rbf = alloc([128, NE])
    PERSIST = ptr[0]
    modT3 = modT.rearrange("p (c s) -> p c s", s=2)

    def gs(which, s, j):
        o = which * 16 + s * 8 + j
        return GS[:, o:o + 1]

    rot = {}

    def rotbuf(name, k, shape, dt=F32):
        if name not in rot:
            rot[name] = [[alloc(shape, dt) for _ in range(k)], 0]
        bufs, i = rot[name]
        rot[name][1] = i + 1
        return bufs[i % k], (name, i % k)

    def norm_group(l, which, t0, n, cx, xt, xkey, hT, hcol, hkeyf):
        for kc in range(8):
            sq, sqk = rotbuf("nsq", 2, [128, 512])
            act(sq[:, 0:n], xt[:, kc, 0:n], AF.Square, r=[xkey], w=[sqk])
            mm(ps[7][:, 0:n], ones_f, sq[:, 0:n], kc == 0, kc == 7, r=[sqk, ("cst",)], w=[("ps", 7)])
        rt, rtk = rotbuf("nrt", 1, [128, 512])
        act(rt[:, 0:n], ps[7][:, 0:n], AF.Sqrt, r=[("ps", 7), ("eps",)], w=[rtk], bias=epsT, scale=1.0 / D)
        recip(rt[:, 0:n], rt[:, 0:n], r=[rtk], w=[rtk])
        for kc in range(8):
            tmp, tk = rotbuf("ntmp", 2, [128, 512])
            stt(tmp[:, 0:n], xt[:, kc, 0:n], gs(which, cx, kc), rt[:, 0:n], ALU.mult, ALU.mult,
                r=[xkey, rtk, ("GS",)], w=[tk])
            act(hT[:, kc, hcol:hcol + n], tmp[:, 0:n], AF.Identity, r=[tk, ("GS",)], w=[hkeyf(kc)],
                bias=gs(which + 1, cx, kc))

    def headnorm_rope(zps, zkey, n, t0, cx, gaincol, cosT, sinT, outb, outk):
        sq, sqk = rotbuf("hsq", 2, [128, 512])
        act(sq[:, 0:n], zps[:, 0:n], AF.Square, r=[zkey], w=[sqk])
        mm(ps[6][:, 0:n], blk64, sq[:, 0:n], True, True, r=[sqk, ("cst",)], w=[("ps", 6)])
        rt, rtk = rotbuf("hrt", 2, [128, 512])
        act(rt[:, 0:n], ps[6][:, 0:n], AF.Sqrt, r=[("ps", 6), ("eps",)], w=[rtk], bias=epsT, scale=1.0 / 64)
        recip(rt[:, 0:n], rt[:, 0:n], r=[rtk], w=[rtk])
        if cx:
            stt(outb[:, 0:n], zps[:, 0:n], pv[:, gaincol:gaincol + 1], rt[:, 0:n], ALU.mult, ALU.mult,
                r=[zkey, rtk, ("pv",)], w=[outk])
            return
        kn, knk = rotbuf("hkn", 2, [128, 512], BF16)
        stt(kn[:, 0:n], zps[:, 0:n], pv[:, gaincol:gaincol + 1], rt[:, 0:n], ALU.mult, ALU.mult,
            r=[zkey, rtk, ("pv",)], w=[knk])
        mm(ps[5][:, 0:n], Rtb, kn[:, 0:n], True, True, r=[knk, ("cstb",)], w=[("ps", 5)])
        t1, t1k = rotbuf("ht1", 2, [128, 512])
        tt("pool", t1[:, 0:n], kn[:, 0:n], cosT[:, t0:t0 + n], ALU.mult, r=[knk, ("rope",)], w=[t1k])
        t2, t2k = rotbuf("ht2", 2, [128, 512])
        tt("dve", t2[:, 0:n], ps[5][:, 0:n], sinT[:, t0:t0 + n], ALU.mult, r=[("ps", 5), ("rope",)], w=[t2k])
        tt("dve", outb[:, 0:n], t1[:, 0:n], t2[:, 0:n], ALU.add, r=[t1k, t2k], w=[outk])

    w_in_v = w_in.rearrange("l (kc p) n -> l p kc n", p=128)
    for (t0, n, cx) in groups:
        xt, xk = rotbuf("xcp", 2, [128, 8, 512])
        dma("sp", xt[:, :, 0:n], xT_in.rearrange("(c p) t -> p c t", p=128)[:, :, t0:t0 + n], r=[], w=[xk])
        dma("pool", xT.rearrange("(c p) t -> p c t", p=128)[:, :, t0:t0 + n], xt[:, :, 0:n], r=[xk], w=[("xT", t0)])
    sc.barrier()
    ptr[0] = PERSIST
    rot.clear()

    for l in range(L):
        last = False
        lam_init = 0.8 - 0.6 * math.exp(-0.3 * (LAYER_IDS[l] if LAYER_IDS else l))
        dma("sp", pv, pv_in[l], r=[], w=[("pv",)])
        ts("dve", pv[:, O_B1P:O_B1P + 512], pv[:, O_B1:O_B1 + 512], 1.0, None, ALU.add, None, r=[("pv",)], w=[("pv",)])
        dma("pool", b2bb[0:NE, :], b2_in[l], r=[], w=[("b2",)])
        dma("sp", rbf[0:1, :], rb_in[l], r=[], w=[("rbf",)])
        sc.op("dve", lambda e: e.tensor_copy(rbb[0:1, :], rbf[0:1, :]), r=[("rbf",)], w=[("rbb",)])
        wm_v = w_mod[l].rearrange("(kc p) n -> p kc n", p=128)
        for g in range(12):
            wt, wk = rotbuf("wmod", 2, [128, 8, 512])
            dma("sp", wt, wm_v[:, :, g * 512:(g + 1) * 512], r=[], w=[wk])
            for cc in range(4):
                ch = g * 4 + cc
                for kc in range(8):
                    mm(ps[0][:, ch * 2:ch * 2 + 2], wt[:, kc, cc * 128:(cc + 1) * 128], silc[:, kc * 2:kc * 2 + 2],
                       kc == 0, kc == 7, r=[wk, ("silc",)], w=[("ps", 0)])
        tt("dve", modT, ps[0][:, 0:96], pv[:, O_BMOD:O_BMOD + 96], ALU.add, r=[("ps", 0), ("pv",)], w=[("modT",)])
        for s in range(2):
            for (which, scc, shc, gac, ng) in ((0, 8, 0, 16, O_N1), (3, 32, 24, 40, O_N2)):
                o = which * 16 + s * 8
                ts("dve", GS[:, o:o + 8], modT3[:, scc:scc + 8, s], 1.0, None, ALU.add, None, r=[("modT",)], w=[("GS",)])
                tt("dve", GS[:, o:o + 8], GS[:, o:o + 8], pv[:, ng:ng + 8], ALU.mult, r=[("GS",), ("pv",)], w=[("GS",)])
                sc.op("dve", (lambda o=o, shc=shc, s=s: (lambda e: e.tensor_copy(GS[:, o + 16:o + 24], modT3[:, shc:shc + 8, s])))(),
                      r=[("modT",)], w=[("GS",)])
                sc.op("dve", (lambda o=o, gac=gac, s=s: (lambda e: e.tensor_copy(GS[:, o + 32:o + 40], modT3[:, gac:gac + 8, s])))(),
                      r=[("modT",)], w=[("GS",)])
        lp, lpk = rotbuf("lamp", 1, [128, 2])
        tt("dve", lp[:, 0:1], pv[:, O_LAM:O_LAM + 1], pv[:, O_LAM + 1:O_LAM + 2], ALU.mult, r=[("pv",)], w=[lpk])
        tt("dve", lp[:, 1:2], pv[:, O_LAM + 2:O_LAM + 3], pv[:, O_LAM + 3:O_LAM + 4], ALU.mult, r=[("pv",), lpk], w=[lpk])
        mm(ps[1][:, 0:2], ones_f, lp, True, True, r=[lpk, ("cst",)], w=[("ps", 1)])
        le, lek = rotbuf("lame", 1, [128, 2])
        act(le, ps[1][:, 0:2], AF.Exp, r=[("ps", 1)], w=[lek])
        tt("dve", lamt[:, 0:1], le[:, 1:2], le[:, 0:1], ALU.subtract, r=[lek], w=[("lamt",)])
        tt("dve", lamt[:, 0:1], lamt[:, 0:1], pv[:, O_LI:O_LI + 1], ALU.add, r=[("lamt",), ("pv",)], w=[("lamt",)])
        tt("dve", lamt[:, 1:2], pv[:, O_SUB:O_SUB + 1], pv[:, O_LI + 1:O_LI + 2], ALU.mult, r=[("pv",)], w=[("lamt",)])
        sc.barrier()
        ptr[0] = PERSIST
        MARK_L = ptr[0]
        rot.clear()

        SG = 4
        sgs = [groups[i:i + SG] for i in range(0, len(groups), SG)]
        cosT = alloc([128, SG * 512])
        sinT = alloc([128, SG * 512])
        hT = alloc([128, 8, SG * 512], BF16)
        MARK_A = ptr[0]
        for sg in sgs:
            ptr[0] = MARK_A
            rot.clear()
            lat = [g_ for g_ in sg if not g_[2]]
            ra = lat[0][0] if lat else 0
            if lat:
                rb_ = lat[-1][0] + 512
                dma("sp", cosT[:, 0:rb_ - ra], cos_in[:, ra:rb_], r=[], w=[("rope",)])
                dma("sp", sinT[:, 0:rb_ - ra], sin_in[:, ra:rb_], r=[], w=[("rope",)])
            cols = []
            c0 = 0
            for (t0, n, cx) in sg:
                cols.append(c0)
                xt, xk = rotbuf("xa", 2, [128, 8, 512])
                dma("sp", xt[:, :, 0:n], xT.rearrange("(c p) t -> p c t", p=128)[:, :, t0:t0 + n], r=[("xT", t0)], w=[xk])
                norm_group(l, 0, t0, n, cx, xt, xk, hT, c0, lambda kc, t0=t0: ("hT", t0, kc))
                c0 += n
            sc.barrier()
            ptr[0] = MARK_A
            rot.clear()

            def hkeys(t0):
                return [("hT", t0)]

            def proj_fm(wt, wk, wc0, sgi, pbank):
                (t0, n, cx) = sg[sgi]
                for kc in range(8):
                    mm(ps[pbank][:, 0:n], wt[:, kc, wc0:wc0 + 128], hT[:, kc, cols[sgi]:cols[sgi] + n], kc == 0, kc == 7,
                       r=[wk] + hkeys(t0), w=[("ps", pbank)])

            def loadw(c0_, ncol, name="wa"):
                wt, wk = rotbuf(name, 2, [128, 8, 1024], BF16)
                dma("pool", wt[:, :, 0:ncol], w_in_v[l][:, :, c0_:c0_ + ncol], r=[], w=[wk])
                return wt, wk

            pb = [0]
            for (kind, wc, nch, gain, dst, drow) in (
                    ("k", 0, 1, O_HG + 1, kT, 0), ("k", 256, 4, O_HG + 3, kT, 128),
                    ("q", 1280, 4, O_HG + 0, qT, 0), ("q", 1792, 4, O_HG + 2, qT, 512)):
                wt, wk = loadw(wc, nch * 128)
                for ch in range(nch):
                    for sgi, (t0, n, cx) in enumerate(sg):
                        bank = pb[0] % 2
                        pb[0] += 1
                        proj_fm(wt, wk, ch * 128, sgi, bank)
                        ob, obk = rotbuf("kqo", 3, [128, 512], BF16)
                        headnorm_rope(ps[bank], ("ps", bank), n, t0 - ra, cx, gain, cosT, sinT, ob, obk)
                        r0 = drow + ch * 128
                        dma("pool", dst[r0:r0 + 128, t0:t0 + n], ob[:, 0:n], r=[obk], w=[(U("kq"),)])
            wv, wvk = loadw(128, 128)
            dma("pool", wv[:, :, 128:640], w_in_v[l][:, :, 768:1280], r=[], w=[wvk])
            for sgi, (t0, n, cx) in enumerate(sg):
                for tl in range(n // 128):
                    vt, vtk = rotbuf("vt", 2, [128, 768], BF16)
                    sc.op("pool", lambda e, vt=vt: e.memset(vt[:, 64:128], 1.0), w=[vtk])
                    sc.op("pool", lambda e, vt=vt: e.memset(vt[:, 192:256], 1.0), w=[vtk])
                    hc = cols[sgi] + tl * 128
                    for kc in range(8):
                        mm(ps[2][:, 0:128], hT[:, kc, hc:hc + 128], wv[:, kc, 0:128], kc == 0, kc == 7,
                           r=[wvk] + hkeys(t0), w=[("ps", 2)])
                    for kc in range(8):
                        mm(ps[3][:, 0:512], hT[:, kc, hc:hc + 128], wv[:, kc, 128:640], kc == 0, kc == 7,
                           r=[wvk] + hkeys(t0), w=[("ps", 3)])
                    sc.op("act", lambda e, vt=vt: e.activation(vt[:, 0:64], ps[2][:, 0:64], AF.Copy), r=[("ps", 2)], w=[vtk])
                    sc.op("act", lambda e, vt=vt: e.activation(vt[:, 128:192], ps[2][:, 64:128], AF.Copy), r=[("ps", 2)], w=[vtk])
                    sc.op("dve", lambda e, vt=vt: e.tensor_copy(vt[:, 256:768], ps[3][:, 0:512]), r=[("ps", 3)], w=[vtk])
                    tk0 = t0 + tl * 128
                    dma("pool", vD[tk0:tk0 + 128, :], vt, r=[vtk], w=[(U("v"),)])
            wt, wk = loadw(2304, 1024)
            for j in range(4):
                for sgi, (t0, n, cx) in enumerate(sg):
                    proj_fm(wt, wk, 512 + j * 128, sgi, 0)
                    sg_, sgk = rotbuf("csig", 2, [128, 512])
                    act(sg_[:, 0:n], ps[0][:, 0:n], AF.Sigmoid, r=[("ps", 0)], w=[sgk])
                    proj_fm(wt, wk, j * 128, sgi, 1)
                    ub, ubk = rotbuf("cu", 2, [128, 512], BF16)
                    tt("dve", ub[:, 0:n], ps[1][:, 0:n], sg_[:, 0:n], ALU.mult, r=[("ps", 1), sgk], w=[ubk])
                    dma("pool", uT[j * 128:(j + 1) * 128, t0:t0 + n], ub[:, 0:n], r=[ubk], w=[(U("u"),)])
            for gb in range(6):
                wt, wk = loadw(3328 + gb * 512, 512)
                for cc in range(4):
                    ch = gb * 4 + cc
                    for sgi, (t0, n, cx) in enumerate(sg):
                        bank = pb[0] % 2
                        pb[0] += 1
                        proj_fm(wt, wk, cc * 128, sgi, bank)
                        gb_, gbk = rotbuf("gsb", 3, [128, 512])
                        act(gb_[:, 0:n], ps[bank][:, 0:n], AF.Sigmoid, r=[("ps", bank), ("pv",)], w=[gbk],
                            bias=pv[:, O_BG + ch:O_BG + ch + 1])
                        dma("pool", gT[ch * 128:(ch + 1) * 128, t0:t0 + n], gb_[:, 0:n], r=[gbk], w=[(U("g"),)])
            sc.barrier()
        ptr[0] = MARK_L
        rot.clear()

        qgroups = [g for g in groups if not (last and g[2])]
        kT3 = None
        Pn = 3
        Pb = None
        pi = [0]
        sbank = [0]
        kt_lat = list(range(NKT))
        kt_ctx = [S // 128, S // 128 + 1]

        def qk_exp(KgT, kkey, QhT, qkey, t0, n, kt):
            bank = sbank[0] % 2
            sbank[0] += 1
            mm(ps[bank][:, 0:n], KgT[:, kt * 128:(kt + 1) * 128], QhT[:, t0:t0 + n], True, True,
               r=[kkey, qkey], w=[("ps", bank)])
            i = pi[0] % Pn
            pi[0] += 1
            act(Pb[i][:, 0:n], ps[bank][:, 0:n], AF.Exp, r=[("ps", bank)], w=[("P", i)], scale=0.125)
            return Pb[i], ("P", i)

        for g in range(2):
            ptr[0] = MARK_L
            rot.clear()
            Pb = [alloc([128, 512], BF16) for _ in range(Pn)]
            KgT = alloc([128, NT], BF16)
            Vg = alloc([128, NKT, 128], BF16)
            dma("sp", KgT[0:64, :], kT[g * 64:(g + 1) * 64, :], r=[], w=[("Kg",)])
            dma("sp", Vg, vD.rearrange("(k p) c -> p k c", p=128)[:, :, g * 128:(g + 1) * 128], r=[], w=[("Vg",)])
            for j in range(4):
                h = g * 4 + j
                QhT, qk_ = rotbuf("Qh", 2, [128, NT], BF16)
                dma("sp", QhT[0:64, :], qT[h * 64:(h + 1) * 64, :], r=[], w=[qk_])
                for (t0, n, cx) in qgroups:
                    kts = kt_ctx if cx else kt_lat
                    for ki, kt in enumerate(kts):
                        P, pk = qk_exp(KgT[0:64, :], ("Kg",), QhT[0:64, :], qk_, t0, n, kt)
                        mm(ps[2][:, 0:n], Vg[:, kt, :], P[:, 0:n], ki == 0, ki == len(kts) - 1,
                           r=[pk, ("Vg",)], w=[("ps", 2)])
                    rcA, rak = rotbuf("rcA", 2, [128, 512])
                    recip(rcA[64:128, 0:n], ps[2][64:128, 0:n], r=[("ps", 2)], w=[rak])
                    rcB, rbk = rotbuf("rcB", 2, [128, 512])
                    dma("sp", rcB[0:64, 0:n], rcA[64:128, 0:n], r=[rak], w=[rbk])
                    ob, obk = rotbuf("ogo", 2, [128, 512], BF16)
                    tt("dve", ob[0:64, 0:n], ps[2][0:64, 0:n], rcB[0:64, 0:n], ALU.mult, r=[("ps", 2), rbk], w=[obk])
                    dma("pool", ogT[h * 64:(h + 1) * 64, t0:t0 + n], ob[0:64, 0:n], r=[obk], w=[(U("og"),)])
            sc.barrier()
        for hd in range(4):
            ptr[0] = MARK_L
            rot.clear()
            Pb = [alloc([128, 512], BF16) for _ in range(Pn)]
            Kd = [alloc([128, NT], BF16) for _ in range(2)]
            Qd = [alloc([128, NT], BF16) for _ in range(2)]
            Vd = alloc([128, NKT, 128], BF16)
            for c in range(2):
                r0 = 128 + (hd * 2 + c) * 64
                dma("sp", Kd[c][0:64, :], kT[r0:r0 + 64, :], r=[], w=[("Kd", c)])
                q0 = 512 + (hd * 2 + c) * 64
                dma("sp", Qd[c][0:64, :], qT[q0:q0 + 64, :], r=[], w=[("Qd", c)])
            dma("sp", Vd, vD.rearrange("(k p) c -> p k c", p=128)[:, :, 256 + hd * 128:256 + (hd + 1) * 128], r=[], w=[("Vd",)])
            for (t0, n, cx) in qgroups:
                kts = kt_ctx if cx else kt_lat
                for c in range(2):
                    for ki, kt in enumerate(kts):
                        P, pk = qk_exp(Kd[c][0:64, :], ("Kd", c), Qd[c][0:64, :], ("Qd", c), t0, n, kt)
                        mm(ps[2 + c][:, 0:n], Vd[:, kt, :], P[:, 0:n], ki == 0, ki == len(kts) - 1,
                           r=[pk, ("Vd",)], w=[("ps", 2 + c)])
                        mm(ps[4 + c][:, 0:n], onesb, P[:, 0:n], ki == 0, ki == len(kts) - 1,
                           r=[pk, ("cstb",)], w=[("ps", 4 + c)])
                ab = []
                for c in range(2):
                    rc, rck = rotbuf("drc", 2, [128, 512])
                    recip(rc[:, 0:n], ps[4 + c][:, 0:n], r=[("ps", 4 + c)], w=[rck])
                    a, ak = rotbuf("da", 2, [128, 512])
                    tt("dve", a[:, 0:n], ps[2 + c][:, 0:n], rc[:, 0:n], ALU.mult, r=[("ps", 2 + c), rck], w=[ak])
                    ab.append((a, ak))
                od, odk = rotbuf("dod", 1, [128, 512])
                stt(od[:, 0:n], ab[1][0][:, 0:n], lamt[:, 0:1], ab[0][0][:, 0:n], ALU.mult, ALU.add,
                    r=[ab[0][1], ab[1][1], ("lamt",)], w=[odk])
                sq, sqk = rotbuf("dsq", 1, [128, 512])
                act(sq[:, 0:n], od[:, 0:n], AF.Square, r=[odk], w=[sqk])
                mm(ps[6][:, 0:n], ones_f, sq[:, 0:n], True, True, r=[sqk, ("cst",)], w=[("ps", 6)])
                rt, rtk = rotbuf("drt", 1, [128, 512])
                act(rt[:, 0:n], ps[6][:, 0:n], AF.Sqrt, r=[("ps", 6), ("eps",)], w=[rtk], bias=epsT, scale=1.0 / 128)
                recip(rt[:, 0:n], rt[:, 0:n], r=[rtk], w=[rtk])
                ob, obk = rotbuf("dob", 2, [128, 512], BF16)
                stt(ob[:, 0:n], od[:, 0:n], lamt[:, 1:2], rt[:, 0:n], ALU.mult, ALU.mult, r=[odk, rtk, ("lamt",)], w=[obk])
                dma("pool", odT[hd * 128:(hd + 1) * 128, t0:t0 + n], ob[:, 0:n], r=[obk], w=[(U("od"),)])
            sc.barrier()
        ptr[0] = MARK_L
        rot.clear()

        dg = alloc([128, 124, 128], BF16)
        for jt in range(124):
            sc.op("dve", lambda e, jt=jt: e.tensor_scalar(dg[:, jt, :], identb, pv[:, O_DW + jt:O_DW + jt + 1], None, ALU.mult),
                  r=[("cstb",), ("pv",)], w=[("dg", jt)])
        pwb = alloc([128, 4, D], BF16)
        wgob = alloc([128, 4, D], BF16)
        wdob = alloc([128, 4, D], BF16)
        woutb = alloc([128, 8, D], BF16)
        dma("pool", pwb, pw_w[l].rearrange("(kc p) n -> p kc n", p=128), r=[], w=[("pwb",)])
        dma("pool", wgob, wgo_w[l].rearrange("(kc p) n -> p kc n", p=128), r=[], w=[("wgob",)])
        dma("pool", wdob, wdo_w[l].rearrange("(kc p) n -> p kc n", p=128), r=[], w=[("wdob",)])
        dma("pool", woutb, wout_w[l].rearrange("(kc p) n -> p kc n", p=128), r=[], w=[("woutb",)])
        for (t0, n, cx) in qgroups:
            seq0, seq1 = (S, NT) if cx else (0, S)
            up, upk = rotbuf("up", 2, [128, 4, 544], BF16)
            sc.op("pool", lambda e, up=up: e.memset(up, 0.0), w=[upk])
            lo = max(seq0, t0 - 15)
            hi = min(seq1, t0 + n + 15)
            o0 = lo - (t0 - 15)
            dma("sp", up[:, :, o0:o0 + (hi - lo)], uT.rearrange("(c p) t -> p c t", p=128)[:, :, lo:hi], r=[], w=[upk])
            cv, cvk = rotbuf("cv", 1, [128, 4, 512])
            sq, sqk = rotbuf("cvsq", 1, [128, 4, 512])
            for j in range(4):
                bank = j % 2
                for t in range(31):
                    mm(ps[bank][:, 0:n], dg[:, j * 31 + t, :], up[:, j, t:t + n], t == 0, t == 30,
                       r=[("dg", j * 31 + t), upk], w=[("ps", bank)])
                act(cv[:, j, 0:n], ps[bank][:, 0:n], AF.Identity, r=[("ps", bank), ("pv",)], w=[cvk + (j,)],
                    bias=pv[:, O_DWB + j:O_DWB + j + 1])
                act(sq[:, j, 0:n], cv[:, j, 0:n], AF.Square, r=[cvk + (j,)], w=[sqk + (j,)])
            for j in range(4):
                mm(ps[2][:, 0:n], ones_f, cv[:, j, 0:n], j == 0, j == 3, r=[cvk + (j,), ("cst",)], w=[("ps", 2)])
            for j in range(4):
                mm(ps[3][:, 0:n], ones_f, sq[:, j, 0:n], j == 0, j == 3, r=[sqk + (j,), ("cst",)], w=[("ps", 3)])
            mean, mk = rotbuf("lnm", 1, [128, 512])
            act(mean[:, 0:n], ps[2][:, 0:n], AF.Copy, r=[("ps", 2)], w=[mk], scale=1.0 / 512)
            msq, msk = rotbuf("lnms", 1, [128, 512])
            tt("dve", msq[:, 0:n], mean[:, 0:n], mean[:, 0:n], ALU.mult, r=[mk], w=[msk])
            var, vk = rotbuf("lnv", 1, [128, 512])
            stt(var[:, 0:n], ps[3][:, 0:n], 1.0 / 512, msq[:, 0:n], ALU.mult, ALU.subtract, r=[("ps", 3), msk], w=[vk])
            act(var[:, 0:n], var[:, 0:n], AF.Sqrt, r=[vk, ("eps",)], w=[vk], bias=epsT)
            recip(var[:, 0:n], var[:, 0:n], r=[vk], w=[vk])
            sT, sTk = rotbuf("sT", 1, [128, 4, 512], BF16)
            for j in range(4):
                d_, dk = rotbuf("lnd", 2, [128, 512])
                tt("dve", d_[:, 0:n], cv[:, j, 0:n], mean[:, 0:n], ALU.subtract, r=[cvk + (j,), mk], w=[dk])
                tt("pool", d_[:, 0:n], d_[:, 0:n], var[:, 0:n], ALU.mult, r=[dk, vk], w=[dk])
                act(sT[:, j, 0:n], d_[:, 0:n], AF.Silu, r=[dk, ("pv",)], w=[sTk + (j,)],
                    bias=pv[:, O_LNB + j:O_LNB + j + 1], scale=pv[:, O_LNG + j:O_LNG + j + 1])
            og, ogk = rotbuf("ogl", 1, [128, 4, 512], BF16)
            od, odk = rotbuf("odl", 1, [128, 4, 512], BF16)
            dma("sp", og[:, :, 0:n], ogT.rearrange("(c p) t -> p c t", p=128)[:, :, t0:t0 + n], r=[], w=[ogk])
            dma("sp", od[:, :, 0:n], odT.rearrange("(c p) t -> p c t", p=128)[:, :, t0:t0 + n], r=[], w=[odk])
            mg, mgk = rotbuf("mg", 1, [128, 8, 512], BF16)
            for oc in range(8):
                gt, gtk = rotbuf("gt", 1, [128, 3, 512])
                dma("sp", gt[:, :, 0:n], gT.rearrange("(b c p) t -> p b c t", b=3, p=128)[:, :, oc, t0:t0 + n], r=[], w=[gtk])
                for bi, (wb, wbk, src, srck) in enumerate(((pwb, ("pwb",), sT, sTk), (wgob, ("wgob",), og, ogk), (wdob, ("wdob",), od, odk))):
                    for j in range(4):
                        mm(ps[4 + bi][:, 0:n], wb[:, j, oc * 128:(oc + 1) * 128], src[:, j, 0:n], j == 0, j == 3,
                           r=[wbk, srck], w=[("ps", 4 + bi)])
                m1, m1k = rotbuf("m1", 2, [128, 512])
                m2, m2k = rotbuf("m2", 2, [128, 512])
                tt("dve", m1[:, 0:n], ps[4][:, 0:n], gt[:, 0, 0:n], ALU.mult, r=[("ps", 4), gtk], w=[m1k])
                tt("dve", m2[:, 0:n], ps[5][:, 0:n], gt[:, 1, 0:n], ALU.mult, r=[("ps", 5), gtk], w=[m2k])
                tt("pool", m1[:, 0:n], m1[:, 0:n], m2[:, 0:n], ALU.add, r=[m1k, m2k], w=[m1k])
                tt("dve", m2[:, 0:n], ps[6][:, 0:n], gt[:, 2, 0:n], ALU.mult, r=[("ps", 6), gtk, m1k], w=[m2k])
                tt("pool", mg[:, oc, 0:n], m1[:, 0:n], m2[:, 0:n], ALU.add, r=[m1k, m2k], w=[mgk + (oc,)])
            xt, xk = rotbuf("xb", 1, [128, 8, 512])
            dma("sp", xt[:, :, 0:n], xT.rearrange("(c p) t -> p c t", p=128)[:, :, t0:t0 + n], r=[("xT", t0)], w=[xk])
            for oc in range(8):
                bank = 7 if oc % 2 == 0 else 3
                for kc in range(8):
                    mm(ps[bank][:, 0:n], woutb[:, kc, oc * 128:(oc + 1) * 128], mg[:, kc, 0:n], kc == 0, kc == 7,
                       r=[("woutb",), mgk], w=[("ps", bank)])
                stt(xt[:, oc, 0:n], ps[bank][:, 0:n], gs(2, cx, oc), xt[:, oc, 0:n], ALU.mult, ALU.add,
                    r=[("ps", bank), ("GS",), xk], w=[xk])
            dma("pool", xT.rearrange("(c p) t -> p c t", p=128)[:, :, t0:t0 + n], xt[:, :, 0:n], r=[xk], w=[("xT", t0)])
        sc.barrier()
        ptr[0] = MARK_L
        rot.clear()

        rwb = alloc([128, 8, NE], BF16)
        dma("pool", rwb, rw_w[l].rearrange("(kc p) n -> p kc n", p=128), r=[], w=[("rwb",)])
        PG = 3
        passes = [qgroups[i:i + PG] for i in range(0, len(qgroups), PG)]
        xm = alloc([128, 8, PG * 512])
        h2 = alloc([128, 8, PG * 512], BF16)
        chi = alloc([128, PG * 512], BF16)
        clo = alloc([128, PG * 512], BF16)
        cbT = alloc([128, PG * 512])
        actT = alloc([128, 8, PG * 512], BF16)
        MARK_C = ptr[0]
        w1v = w1_w[l].rearrange("e (kc p) n -> e p kc n", p=128)
        w2v = w2_w[l].rearrange("e (j p) n -> e p j n", p=128)
        for pss in passes:
            ptr[0] = MARK_C
            rot.clear()
            cols = []
            c0 = 0
            for (t0, n, cx) in pss:
                cols.append(c0)
                dma("sp", xm[:, :, c0:c0 + n], xT.rearrange("(c p) t -> p c t", p=128)[:, :, t0:t0 + n], r=[("xT", t0)], w=[("xm", t0)])
                norm_group(l, 3, t0, n, cx, xm[:, :, c0:c0 + n], ("xm", t0), h2, c0, lambda kc, t0=t0: ("h2", t0, kc))
                for tl in range(n // 128):
                    hc = c0 + tl * 128
                    for kc in range(8):
                        mm(ps[0][:, 0:NE], h2[:, kc, hc:hc + 128], rwb[:, kc, :], kc == 0, False,
                           r=[("h2", t0), ("rwb",)], w=[("ps", 0)])
                    mm(ps[0][:, 0:NE], onesb[0:1, :], rbb[0:1, :], False, True, r=[("cstb",), ("rbb",)], w=[("ps", 0)])
                    lg, lgk = rotbuf("lg", 2, [128, NE])
                    act(lg, ps[0][:, 0:NE], AF.Copy, r=[("ps", 0)], w=[lgk])
                    t8, t8k = rotbuf("t8", 2, [128, 8])
                    sc.op("dve", lambda e, t8=t8, lg=lg: e.max(t8, lg), r=[lgk], w=[t8k])
                    mk_, mkk = rotbuf("rmask", 2, [128, NE])
                    ts("dve", mk_, lg, t8[:, 3:4], None, ALU.is_ge, None, r=[lgk, t8k], w=[mkk])
                    ng, ngk = rotbuf("rneg", 2, [128, 1])
                    ts("dve", ng, t8[:, 0:1], -1.0, None, ALU.mult, None, r=[t8k], w=[ngk])
                    ex, exk = rotbuf("rex", 2, [128, NE])
                    act(ex, lg, AF.Exp, r=[lgk, ngk], w=[exk], bias=ng)
                    tt("dve", ex, ex, mk_, ALU.mult, r=[exk, mkk], w=[exk])
                    ssum, ssk = rotbuf("rsum", 2, [128, 1])
                    sc.op("dve", lambda e, ssum=ssum, ex=ex: e.reduce_sum(ssum, ex, AX.X), r=[exk], w=[ssk])
                    recip(ssum, ssum, r=[ssk], w=[ssk])
                    ts("dve", ex, ex, ssum, None, ALU.mult, None, r=[exk, ssk], w=[exk])
                    sc.op("pe", lambda e, ex=ex: e.transpose(ps[1][0:NE, 0:128], ex, ident), r=[exk, ("cst",)], w=[("ps", 1)])
                    act(chi[0:NE, hc:hc + 128], ps[1][0:NE, 0:128], AF.Copy, r=[("ps", 1)], w=[("chi", t0, tl)])
                    tt("dve", clo[0:NE, hc:hc + 128], ps[1][0:NE, 0:128], chi[0:NE, hc:hc + 128], ALU.subtract,
                       r=[("ps", 1), ("chi", t0, tl)], w=[("clo", t0, tl)])
                for oc in range(8):
                    bank = 2 + oc % 2
                    mm(ps[bank][:, 0:n], b2bb[0:NE, oc * 128:(oc + 1) * 128], chi[0:NE, c0:c0 + n], True, False,
                       r=[("b2",), ("chi", t0)], w=[("ps", bank)])
                    mm(ps[bank][:, 0:n], b2bb[0:NE, oc * 128:(oc + 1) * 128], clo[0:NE, c0:c0 + n], False, True,
                       r=[("b2",), ("clo", t0)], w=[("ps", bank)])
                    stt(xm[:, oc, c0:c0 + n], ps[bank][:, 0:n], gs(5, cx, oc), xm[:, oc, c0:c0 + n], ALU.mult, ALU.add,
                        r=[("ps", bank), ("GS",), ("xm", t0)], w=[("xm", t0)])
                c0 += n
            for ex_i in range(NE):
                for gi, (t0, n, cx) in enumerate(pss):
                    c0 = cols[gi]
                    mm(ps[7][:, 0:n], selc3[0:NE, ex_i, :], chi[0:NE, c0:c0 + n], True, False,
                       r=[("selc",), ("chi", t0)], w=[("ps", 7)])
                    mm(ps[7][:, 0:n], selc3[0:NE, ex_i, :], clo[0:NE, c0:c0 + n], False, True,
                       r=[("selc",), ("clo", t0)], w=[("ps", 7)])
                    act(cbT[:, c0:c0 + n], ps[7][:, 0:n], AF.Copy, r=[("ps", 7)], w=[("cbT", t0)])
                for j in range(8):
                    w1s, w1k = rotbuf("w1s", 4, [128, 8, 256], BF16)
                    dma("pool", w1s[:, :, 0:128], w1v[ex_i][:, :, j * 128:(j + 1) * 128], r=[], w=[w1k])
                    dma("pool", w1s[:, :, 128:256], w1v[ex_i][:, :, D + j * 128:D + (j + 1) * 128], r=[], w=[w1k])
                    for gi, (t0, n, cx) in enumerate(pss):
                        c0 = cols[gi]
                        bg = 0 + (gi % 2) * 2
                        bl = 1 + (gi % 2) * 2
                        for kc in range(8):
                            mm(ps[bg][:, 0:n], w1s[:, kc, 0:128], h2[:, kc, c0:c0 + n], kc == 0, kc == 7,
                               r=[w1k, ("h2", t0)], w=[("ps", bg)])
                        for kc in range(8):
                            mm(ps[bl][:, 0:n], w1s[:, kc, 128:256], h2[:, kc, c0:c0 + n], kc == 0, kc == 7,
                               r=[w1k, ("h2", t0)], w=[("ps", bl)])
                        gcl, gk_ = rotbuf("gcl", 2, [128, 512])
                        bcol = O_B1 + ex_i * 16 + j
                        ts("dve", gcl[:, 0:n], ps[bg][:, 0:n], pv[:, bcol:bcol + 1], 7.0, ALU.add, ALU.min,
                           r=[("ps", bg), ("pv",)], w=[gk_])
                        sg_, sgk = rotbuf("msg", 2, [128, 512])
                        act(sg_[:, 0:n], gcl[:, 0:n], AF.Sigmoid, r=[gk_], w=[sgk], scale=1.702)
                        tl_, tlk = rotbuf("mtl", 2, [128, 512])
                        pcol = O_B1P + ex_i * 16 + 8 + j
                        act(tl_[:, 0:n], ps[bl][:, 0:n], AF.Identity, r=[("ps", bl), ("pv",)], w=[tlk], bias=pv[:, pcol:pcol + 1])
                        ts("dve", tl_[:, 0:n], tl_[:, 0:n], 8.0, -6.0, ALU.min, ALU.max, r=[tlk], w=[tlk])
                        tt("pool", gcl[:, 0:n], gcl[:, 0:n], sg_[:, 0:n], ALU.mult, r=[gk_, sgk], w=[gk_])
                        tt("pool", tl_[:, 0:n], tl_[:, 0:n], cbT[:, c0:c0 + n], ALU.mult, r=[tlk, ("cbT", t0)], w=[tlk])
                        tt("dve", actT[:, j, c0:c0 + n], gcl[:, 0:n], tl_[:, 0:n], ALU.mult, r=[gk_, tlk], w=[("actT", t0, j)])
                for oc in range(8):
                    w2s, w2k = rotbuf("w2s", 4, [128, 8, 128], BF16)
                    dma("pool", w2s, w2v[ex_i][:, :, oc * 128:(oc + 1) * 128], r=[], w=[w2k])
                    for gi, (t0, n, cx) in enumerate(pss):
                        c0 = cols[gi]
                        bank = 4 + gi % 2
                        for j in range(8):
                            mm(ps[bank][:, 0:n], w2s[:, j, :], actT[:, j, c0:c0 + n], j == 0, j == 7,
                               r=[w2k, ("actT", t0, j)], w=[("ps", bank)])
                        stt(xm[:, oc, c0:c0 + n], ps[bank][:, 0:n], gs(5, cx, oc), xm[:, oc, c0:c0 + n], ALU.mult, ALU.add,
                            r=[("ps", bank), ("GS",), ("xm", t0)], w=[("xm", t0)])
            for gi, (t0, n, cx) in enumerate(pss):
                c0 = cols[gi]
                if l == L - 1:
                    dma("pool", out_d.rearrange("(c p) t -> p c t", p=128)[:, :, t0:t0 + n], xm[:, :, c0:c0 + n],
                        r=[("xm", t0)], w=[("out", t0)])
                else:
                    dma("pool", xT.rearrange("(c p) t -> p c t", p=128)[:, :, t0:t0 + n], xm[:, :, c0:c0 + n],
                        r=[("xm", t0)], w=[("xT", t0)])
            sc.barrier()
        ptr[0] = MARK_L
        rot.clear()
        ptr[0] = PERSIST

    sc.barrier()
    sc.emit()
    st.close()
    return nc


def _cols(v):
    v = np.asarray(v, np.float32)
    return np.ascontiguousarray(v.reshape(-1, 128).T)


def _consts(S):
    ident = np.eye(128, dtype=np.float32)
    blk = np.zeros((128, 128), np.float32)
    blk[:64, :64] = 1
    blk[64:, 64:] = 1
    ones = np.ones((128, 128), np.float32)
    R = np.zeros((128, 128), np.float32)
    for m in range(128):
        if m % 32 < 16:
            R[m, m + 16] = -1.0
        else:
            R[m, m - 16] = 1.0
    cst = np.concatenate([ident, blk, ones, np.ascontiguousarray(R.T)], axis=1)
    sel = np.zeros((NE, NE, 128), np.float32)
    for e in range(NE):
        sel[e, e, :] = 1.0
    t = np.arange(S)
    t_row = (t // 64).astype(np.float32)
    t_col = (t % 64).astype(np.float32)
    inv = (np.float32(10000.0) ** (-np.arange(16, dtype=np.float32) / np.float32(16))).astype(np.float32)
    cosT = np.zeros((128, S), np.float32)
    sinT = np.zeros((128, S), np.float32)
    for p in range(128):
        d = p % 64
        pos = t_row if d < 32 else t_col
        ang = (pos * inv[d % 16]).astype(np.float32)
        cosT[p] = np.cos(ang)
        sinT[p] = np.sin(ang)
    return cst, sel.reshape(NE, NE * 128), cosT, sinT


def _pack_pv(inp, l):
    pvl = np.zeros((128, NPV), np.float32)
    bm = _cols(inp["b_mod"][l])
    pvl[:, O_BMOD:O_BMOD + 96] = np.repeat(bm, 2, axis=1)
    pvl[:, O_N1:O_N1 + 8] = _cols(inp["norm1_g"][l])
    pvl[:, O_N2:O_N2 + 8] = _cols(inp["norm2_g"][l])
    pvl[:, O_BG:O_BG + 24] = _cols(inp["b_gate"][l])
    pvl[:, O_DWB:O_DWB + 4] = _cols(inp["conv_dw_b"][l])
    pvl[:, O_LNG:O_LNG + 4] = _cols(inp["conv_ln_g"][l])
    pvl[:, O_LNB:O_LNB + 4] = _cols(inp["conv_ln_b"][l])
    dw = np.asarray(inp["conv_dw_w"][l], np.float32)
    pvl[:, O_DW:O_DW + 124] = dw.T.reshape(4, 128, 31).transpose(1, 0, 2).reshape(128, 124)
    for i, k in enumerate(("gqa_q_norm", "gqa_k_norm", "diff_q_norm", "diff_k_norm")):
        pvl[:, O_HG + i] = np.tile(np.asarray(inp[k][l], np.float32), 2)
    pvl[:, O_SUB] = np.asarray(inp["diff_subln_g"][l], np.float32)
    for i, k in enumerate(("lam_q1", "lam_k1", "lam_q2", "lam_k2")):
        pvl[:64, O_LAM + i] = np.asarray(inp[k][l], np.float32)
    lam_init = 0.8 - 0.6 * math.exp(-0.3 * l)
    pvl[:, O_LI] = -lam_init
    pvl[:, O_LI + 1] = 1.0 - lam_init
    b1 = np.asarray(inp["exp_b1"][l], np.float32)
    b1c = b1.reshape(NE, 16, 128).transpose(2, 0, 1).reshape(128, NE * 16)
    pvl[:, O_B1:O_B1 + 512] = b1c
    return pvl


def kernel(**inp):
    x = np.asarray(inp["x"], np.float32)
    B, S, _ = x.shape
    L = np.asarray(inp["w_mod"]).shape[0]
    nc = build(S, 1)
    cst, sel, cosT, sinT = _consts(S)
    c_ctx = np.asarray(inp["c_ctx"], np.float32)
    ncores = B
    xs = [np.ascontiguousarray(np.concatenate([x[b].T, np.asarray(inp["ctx"][b], np.float32).T], axis=1)) for b in range(B)]
    cts = [np.ascontiguousarray(np.stack([_cols(inp["c"][b]), _cols(c_ctx)], axis=2).reshape(128, 16)) for b in range(B)]
    for l in range(L):
        shared = {
            "pv": _pack_pv(inp, l)[None],
            "rb": np.asarray(inp["router_b"][l], np.float32).reshape(1, 1, NE),
            "b2": np.asarray(inp["exp_b2"][l], np.float32)[None],
            "cosT": cosT, "sinT": sinT, "cst": cst, "selc": sel,
        }
        for k in ("w_mod", "w_in", "conv_pw_w", "w_gqa_o", "w_diff_o", "w_out", "router_w", "exp_w1", "exp_w2"):
            shared[k] = np.asarray(inp[k][l], np.float32)[None]
        in_maps = []
        for b in range(ncores):
            m = dict(shared)
            m["xT0"] = xs[b]
            m["cT"] = cts[b]
            in_maps.append(m)
        res = run_bass_kernel_spmd(nc, in_maps, core_ids=list(range(ncores)))
        xs = [np.ascontiguousarray(res.results[b]["outT"]) for b in range(ncores)]
    out = np.stack([np.ascontiguousarray(xs[b][:, :S].T) for b in range(B)])
    return out.astype(np.float32)
```
